# Optimizing a Trainium2 kernel written in Bass

```python
import math
import jax, jax.numpy as jnp
from jax import lax
import numpy as np

D_MODEL = 1024
BATCH = 2
SEQ = 8192
DEPTH = 2

F32 = jnp.float32
NEG_BIG = -1e30
LOG_TINY = 1e-30
A_HEADS = 8
A_HEAD_DIM = 64
IDX_HEADS = 8
IDX_DIM = 64
IDX_TOPK = 256
Q_BLOCK = 128
B_HEADS = 4
B_KEY_DIM = 128
B_VAL_DIM = 128
B_CHUNK = 64
C_Q_HEADS = 8
C_KV_HEADS = 2
C_HEAD_DIM = 64
WINDOW = 128
BRANCH_WIDTH = 512
N_BRANCHES = 3
N_BUCKETS = 32
MAX_DISTANCE = 128
MEM_LEN = 256
MEM_HEADS = 4
MEM_HEAD_DIM = D_MODEL // MEM_HEADS
N_GROUPS = 4
EXPERTS_PER_GROUP = 8
N_EXPERTS = N_GROUPS * EXPERTS_PER_GROUP
EXPERT_TOPK = 2
D_EXPERT = 512
MOE_BLOCK = 128
DEEPNORM_ALPHA = (2 * DEPTH) ** 0.25
DEEPNORM_BETA = (8 * DEPTH) ** -0.25
LN_EPS = 1e-5
RMS_EPS = 1e-6

SPLITS = (
    A_HEADS * A_HEAD_DIM,
    A_HEADS * A_HEAD_DIM,
    A_HEADS * A_HEAD_DIM,
    IDX_HEADS * IDX_DIM,
    IDX_DIM,
    IDX_HEADS,
    B_HEADS * B_KEY_DIM,
    B_HEADS * B_KEY_DIM,
    B_HEADS * B_VAL_DIM,
    B_HEADS * B_VAL_DIM,
    C_Q_HEADS * C_HEAD_DIM,
    C_KV_HEADS * C_HEAD_DIM,
    C_KV_HEADS * C_HEAD_DIM,
    N_BRANCHES * D_MODEL,
)
IN_WIDTH = sum(SPLITS)

kernel_name = 'hybrid_dsa_hgrn2_swa_hmoe_deepnorm'


def layer_norm(x, g, b):
    xf = x.astype(F32)
    mu = jnp.mean(xf, axis=-1, keepdims=True)
    var = jnp.mean(jnp.square(xf - mu), axis=-1, keepdims=True)
    return ((xf - mu) * lax.rsqrt(var + LN_EPS) * g.astype(F32) + b.astype(F32)).astype(x.dtype)


def t5_bucket(dist):
    exact = N_BUCKETS // 2
    d = dist.astype(F32)
    large = exact + (jnp.log(jnp.maximum(d, float(exact)) / exact) / math.log(MAX_DISTANCE / exact)
                     * (N_BUCKETS - exact)).astype(jnp.int32)
    large = jnp.minimum(large, N_BUCKETS - 1)
    return jnp.where(dist < exact, dist, large)


def dsa_attention(q, k, v, iq, ik, iw, bias_table):
    bsz, s = q.shape[0], q.shape[1]
    nb = s // Q_BLOCK
    topk = min(IDX_TOPK, s // 4)
    spos = jnp.arange(s)
    iq = iq * (IDX_DIM ** -0.5)
    iw = iw * (IDX_HEADS ** -0.5)
    scale = A_HEAD_DIM ** -0.5
    gather = jax.vmap(lambda a, i: a[i])

    def blocks(a):
        return a.reshape(bsz, nb, Q_BLOCK, *a.shape[2:]).swapaxes(0, 1)

    def one_block(args):
        qb, iqb, iwb, n = args
        tpos = n * Q_BLOCK + jnp.arange(Q_BLOCK)
        score = jnp.einsum('bths,bth->bts', jax.nn.relu(jnp.einsum('bthd,bsd->bths', iqb, ik)), iwb).astype(F32)
        score = jnp.where((spos[None, :] <= tpos[:, None])[None], score, NEG_BIG)
        _, idx = lax.top_k(score, topk)
        flat = idx.reshape(bsz, Q_BLOCK * topk)
        ks = gather(k, flat).reshape(bsz, Q_BLOCK, topk, A_HEADS, A_HEAD_DIM)
        vs = gather(v, flat).reshape(bsz, Q_BLOCK, topk, A_HEADS, A_HEAD_DIM)
        dist = tpos[None, :, None] - idx
        bias = bias_table[t5_bucket(jnp.maximum(dist, 0))]
        logits = jnp.einsum('bthd,btkhd->btkh', qb, ks).astype(F32) * scale + bias.astype(F32)
        logits = jnp.where((dist >= 0)[..., None], logits, NEG_BIG)
        p = jax.nn.softmax(logits, axis=2).astype(v.dtype)
        return jnp.einsum('btkh,btkhd->bthd', p, vs)

    out = lax.map(one_block, (blocks(q), blocks(iq), blocks(iw), jnp.arange(nb)))
    return out.swapaxes(0, 1).reshape(bsz, s, A_HEADS * A_HEAD_DIM)


def hgrn2_scan(q, logf, v):
    bsz, s, h, dk = q.shape
    dv = v.shape[-1]
    nc = s // B_CHUNK
    k = -jnp.expm1(logf)

    def to_chunks(a):
        return a.reshape(bsz, nc, B_CHUNK, h, a.shape[-1]).transpose(1, 0, 3, 2, 4)

    causal = jnp.tril(jnp.ones((B_CHUNK, B_CHUNK), dtype=bool))

    def step(state, inp):
        qc, gc, kc, vc = inp
        a = jnp.cumsum(gc, axis=2)
        diff = a[:, :, :, None, :] - a[:, :, None, :, :]
        decay = jnp.exp(jnp.where(causal[:, :, None], diff, NEG_BIG))
        attn = jnp.einsum('bhtd,bhtsd,bhsd->bhts', qc, decay, kc)
        o = jnp.einsum('bhts,bhsv->bhtv', attn, vc) + jnp.einsum('bhtd,bhdv->bhtv', qc * jnp.exp(a), state)
        a_last = a[:, :, -1:, :]
        new_state = jnp.exp(a_last[:, :, 0, :])[..., None] * state + jnp.einsum(
            'bhsd,bhsv->bhdv', kc * jnp.exp(a_last - a), vc)
        return new_state, o

    init = jnp.zeros((bsz, h, dk, dv), F32)
    _, o = lax.scan(step, init, (to_chunks(q), to_chunks(logf), to_chunks(k), to_chunks(v)))
    return o.transpose(1, 0, 3, 2, 4).reshape(bsz, s, h, dv)


def hgrn2_branch(q, f, i, g, lb, norm_gain):
    bsz, s, _ = q.shape
    qh = jax.nn.silu(q.astype(F32)).reshape(bsz, s, B_HEADS, B_KEY_DIM)
    fz = f.astype(F32).reshape(bsz, s, B_HEADS, B_KEY_DIM)
    lbh = jnp.clip(lb.astype(F32), LOG_TINY, 1.0 - 1e-6).reshape(B_HEADS, B_KEY_DIM)
    logf = jnp.logaddexp(jnp.log(lbh), jnp.log1p(-lbh) + jax.nn.log_sigmoid(fz))
    vh = i.astype(F32).reshape(bsz, s, B_HEADS, B_VAL_DIM)
    o = hgrn2_scan(qh, logf, vh)
    o = o * lax.rsqrt(jnp.mean(jnp.square(o), axis=-1, keepdims=True) + RMS_EPS)
    o = o.reshape(bsz, s, B_HEADS * B_VAL_DIM) * norm_gain.astype(F32) * jax.nn.silu(g.astype(F32))
    return o.astype(q.dtype)


def swa_sink_attention(q, k, v, sinks, bias_table):
    bsz, s = q.shape[0], q.shape[1]
    nb = s // WINDOW
    grp = C_Q_HEADS // C_KV_HEADS
    qb = q.reshape(bsz, nb, WINDOW, C_KV_HEADS, grp, C_HEAD_DIM)

    def band(a):
        a = a.reshape(bsz, nb, WINDOW, C_KV_HEADS, C_HEAD_DIM)
        prev = jnp.concatenate([jnp.zeros_like(a[:, :1]), a[:, :-1]], axis=1)
        return jnp.concatenate([prev, a], axis=2)

    kb, vb = band(k), band(v)
    qi = jnp.arange(WINDOW)[:, None]
    kj = jnp.arange(2 * WINDOW)[None, :]
    dist = qi + WINDOW - kj
    kpos = (jnp.arange(nb)[:, None, None] - 1) * WINDOW + kj[None]
    mask = ((dist >= 0) & (dist < WINDOW))[None] & (kpos >= 0)
    bias = bias_table[t5_bucket(jnp.maximum(dist, 0))]
    bias = bias.reshape(WINDOW, 2 * WINDOW, C_KV_HEADS, grp).transpose(2, 3, 0, 1).astype(F32)
    logits = jnp.einsum('bnqhgd,bnkhd->bnhgqk', qb, kb).astype(F32) * (C_HEAD_DIM ** -0.5) + bias
    logits = jnp.where(mask[None, :, None, None], logits, NEG_BIG)
    sink = jnp.broadcast_to(sinks.astype(F32).reshape(C_KV_HEADS, grp, 1, 1), logits.shape[:-1] + (1,))
    p = jax.nn.softmax(jnp.concatenate([logits, sink], axis=-1), axis=-1)[..., :-1].astype(v.dtype)
    out = jnp.einsum('bnhgqk,bnkhd->bnqhgd', p, vb)
    return out.reshape(bsz, s, C_Q_HEADS * C_HEAD_DIM)


def hybrid_mixer(x, w_in, w_branch, w_out, lb, norm_gain, sinks, rel_bias):
    bsz, s, _ = x.shape
    proj = x @ w_in
    offsets = np.cumsum(SPLITS)[:-1].tolist()
    (a_q, a_k, a_v, i_q, i_k, i_w, b_q, b_f, b_i, b_g, c_q, c_k, c_v, gate) = jnp.split(proj, offsets, axis=-1)

    def heads(t, h):
        return t.reshape(bsz, s, h, -1)

    ya = dsa_attention(heads(a_q, A_HEADS), heads(a_k, A_HEADS), heads(a_v, A_HEADS),
                       heads(i_q, IDX_HEADS), i_k, i_w, rel_bias[:, :A_HEADS])
    yb = hgrn2_branch(b_q, b_f, b_i, b_g, lb, norm_gain)
    yc = swa_sink_attention(heads(c_q, C_Q_HEADS), heads(c_k, C_KV_HEADS), heads(c_v, C_KV_HEADS),
                            sinks, rel_bias[:, A_HEADS:])
    branches = jnp.stack([ya, yb, yc], axis=2)
    up = jnp.einsum('bsgc,gcd->bsgd', branches, w_branch)
    gates = jax.nn.sigmoid(gate.reshape(bsz, s, N_BRANCHES, D_MODEL))
    merged = jnp.einsum('bsgd,bsgd->bsd', gates, up)
    return merged @ w_out


def memory_cross_attention(x, mem, wq, wkv, wo):
    bsz, s, _ = x.shape
    m = mem.shape[1]
    q = (x @ wq).reshape(bsz, s, MEM_HEADS, MEM_HEAD_DIM)
    kv = (mem @ wkv).reshape(bsz, m, 2, MEM_HEADS, MEM_HEAD_DIM)
    logits = jnp.einsum('bshd,bmhd->bhsm', q, kv[:, :, 0]).astype(F32) * (MEM_HEAD_DIM ** -0.5)
    p = jax.nn.softmax(logits, axis=-1).astype(x.dtype)
    o = jnp.einsum('bhsm,bmhd->bshd', p, kv[:, :, 1]).reshape(bsz, s, D_MODEL)
    return o @ wo


def routed_expert_mlps(xf, expert, weight, w1, w3, w2):
    n_tok, d = xf.shape
    n_asg = expert.shape[0]
    tok = jnp.arange(n_asg) // EXPERT_TOPK
    order = jnp.argsort(expert)
    se, stok, sw = expert[order], tok[order], weight[order]
    counts = jnp.bincount(expert, length=N_EXPERTS)
    starts = jnp.cumsum(counts) - counts
    padded = (counts + MOE_BLOCK - 1) // MOE_BLOCK * MOE_BLOCK
    pad_ends = jnp.cumsum(padded)
    pad_starts = pad_ends - padded
    dest = pad_starts[se] + jnp.arange(n_asg) - starts[se]
    n_slot = n_asg + N_EXPERTS * MOE_BLOCK
    slot_tok = jnp.zeros((n_slot,), jnp.int32).at[dest].set(stok)
    slot_w = jnp.zeros((n_slot,), weight.dtype).at[dest].set(sw)
    n_blk = n_slot // MOE_BLOCK
    blk_expert = jnp.minimum(jnp.searchsorted(pad_ends, jnp.arange(n_blk) * MOE_BLOCK, side='right'),
                             N_EXPERTS - 1)
    xs = xf[slot_tok].reshape(n_blk, MOE_BLOCK, d)

    def expert_block(args):
        xb, e = args
        hdn = jax.nn.silu(xb @ w1[e]) * (xb @ w3[e])
        return hdn @ w2[e]

    ys = lax.map(expert_block, (xs, blk_expert)).reshape(n_slot, d)
    return jax.ops.segment_sum(ys * slot_w[:, None].astype(ys.dtype), slot_tok, num_segments=n_tok)


def hierarchical_moe(x, wg, bg, we, be, w1, w3, w2):
    bsz, s, d = x.shape
    n_tok = bsz * s
    xf = x.reshape(n_tok, d)
    group_logits = (xf @ wg).astype(F32) + bg.astype(F32)
    grp = jnp.argmax(group_logits, axis=-1)
    p_grp = jnp.take_along_axis(jax.nn.softmax(group_logits, axis=-1), grp[:, None], axis=-1)
    exp_logits = ((xf @ we).astype(F32) + be.astype(F32)).reshape(n_tok, N_GROUPS, EXPERTS_PER_GROUP)
    in_grp = jnp.take_along_axis(exp_logits, grp[:, None, None], axis=1)[:, 0]
    top_val, top_idx = lax.top_k(in_grp, EXPERT_TOPK)
    gate_w = jax.nn.softmax(top_val, axis=-1) * p_grp
    expert = grp[:, None] * EXPERTS_PER_GROUP + top_idx
    y = routed_expert_mlps(xf, expert.reshape(-1), gate_w.reshape(-1).astype(x.dtype), w1, w3, w2)
    return y.reshape(bsz, s, d)


def setup_inputs(seed: int = 0) -> dict:
    key = jax.random.key(seed)
    ks = jax.random.split(key, 22)

    def nrm(k, shape, scale):
        return jax.random.normal(k, shape, jnp.float32) * scale

    return {
        'x': nrm(ks[0], (BATCH, SEQ, D_MODEL), 1.0),
        'mem': nrm(ks[1], (BATCH, MEM_LEN, D_MODEL), 1.0),
        'rel_bias': nrm(ks[2], (N_BUCKETS, A_HEADS + C_Q_HEADS), 0.5),
        'w_in': nrm(ks[3], (DEPTH, D_MODEL, IN_WIDTH), D_MODEL ** -0.5),
        'w_branch': nrm(ks[4], (DEPTH, N_BRANCHES, BRANCH_WIDTH, D_MODEL), BRANCH_WIDTH ** -0.5),
        'w_out': nrm(ks[5], (DEPTH, D_MODEL, D_MODEL), DEEPNORM_BETA * D_MODEL ** -0.5),
        'hgrn_lb': 1.0 + nrm(ks[6], (DEPTH, B_HEADS * B_KEY_DIM), 0.5),
        'hgrn_norm_g': 1.0 + nrm(ks[7], (DEPTH, B_HEADS * B_VAL_DIM), 0.02),
        'c_sinks': nrm(ks[8], (DEPTH, C_Q_HEADS), 0.5),
        'xa_wq': nrm(ks[9], (DEPTH, D_MODEL, D_MODEL), D_MODEL ** -0.5),
        'xa_wkv': nrm(ks[10], (DEPTH, D_MODEL, 2 * D_MODEL), D_MODEL ** -0.5),
        'xa_wo': nrm(ks[11], (DEPTH, D_MODEL, D_MODEL), DEEPNORM_BETA * D_MODEL ** -0.5),
        'moe_wg': nrm(ks[12], (DEPTH, D_MODEL, N_GROUPS), D_MODEL ** -0.5),
        'moe_bg': nrm(ks[13], (DEPTH, N_GROUPS), 0.01),
        'moe_we': nrm(ks[14], (DEPTH, D_MODEL, N_EXPERTS), D_MODEL ** -0.5),
        'moe_be': nrm(ks[15], (DEPTH, N_EXPERTS), 0.01),
        'moe_w1': nrm(ks[16], (DEPTH, N_EXPERTS, D_MODEL, D_EXPERT), D_MODEL ** -0.5),
        'moe_w3': nrm(ks[17], (DEPTH, N_EXPERTS, D_MODEL, D_EXPERT), D_MODEL ** -0.5),
        'moe_w2': nrm(ks[18], (DEPTH, N_EXPERTS, D_EXPERT, D_MODEL), DEEPNORM_BETA * D_EXPERT ** -0.5),
        'ln_g': 1.0 + nrm(ks[19], (DEPTH, 3, D_MODEL), 0.02),
        'ln_b': nrm(ks[20], (DEPTH, 3, D_MODEL), 0.02),
    }


def reference(x, mem, rel_bias, w_in, w_branch, w_out, hgrn_lb, hgrn_norm_g, c_sinks,
              xa_wq, xa_wkv, xa_wo, moe_wg, moe_bg, moe_we, moe_be, moe_w1, moe_w3, moe_w2,
              ln_g, ln_b):
    lb_soft = jax.nn.softmax(hgrn_lb.astype(F32), axis=0)
    lower_bounds = jnp.cumsum(lb_soft, axis=0) - lb_soft[0]
    h = x
    for l in range(DEPTH):
        h = layer_norm(DEEPNORM_ALPHA * h + hybrid_mixer(h, w_in[l], w_branch[l], w_out[l], lower_bounds[l],
                                                         hgrn_norm_g[l], c_sinks[l], rel_bias),
                       ln_g[l, 0], ln_b[l, 0])
        h = layer_norm(DEEPNORM_ALPHA * h + memory_cross_attention(h, mem, xa_wq[l], xa_wkv[l], xa_wo[l]),
                       ln_g[l, 1], ln_b[l, 1])
        h = layer_norm(DEEPNORM_ALPHA * h + hierarchical_moe(h, moe_wg[l], moe_bg[l], moe_we[l], moe_be[l],
                                                             moe_w1[l], moe_w3[l], moe_w2[l]),
                       ln_g[l, 2], ln_b[l, 2])
    return h
```

```python
import math
from contextlib import ExitStack

import numpy as np
import concourse.bass as bass
import concourse.mybir as mybir
from concourse.bass_utils import run_bass_kernel_spmd

F32 = mybir.dt.float32
BF16 = mybir.dt.bfloat16
AF = mybir.ActivationFunctionType
ALU = mybir.AluOpType
AX = mybir.AxisListType

D = 1024
NEGB = -30000.0
OFF = dict(a_q=0, a_k=512, a_v=1024, i_q=1536, i_k=2048, i_w=2112, b_q=2120, b_f=2632,
           b_i=3144, b_g=3656, c_q=4168, c_k=4680, c_v=4808, gate=4936)
ALPHA = (2 * 2) ** 0.25
LN_EPS = 1e-5
RMS_EPS = 1e-6


class _Reg:
    __slots__ = ("lastw", "readers")

    def __init__(self):
        self.lastw = None
        self.readers = []


class TT:
    def __init__(self, ap, name):
        self.ap = ap
        self.name = name
        self.all = _Reg()
        self.regs = {}
        self.excl = False

    def reg(self, label):
        r = self.regs.get(label)
        if r is None:
            r = _Reg()
            r.lastw = self.all.lastw
            r.readers = list(self.all.readers)
            self.regs[label] = r
        return r

    def __getitem__(self, k):
        return self.ap[k]


class _Ins:
    __slots__ = ("eng", "fn", "waits", "needed", "semval", "is_dma", "dsem", "dval", "idx")

    def __init__(self, eng, fn):
        self.eng = eng
        self.fn = fn
        self.waits = []
        self.needed = False
        self.semval = None
        self.is_dma = False
        self.dsem = None
        self.dval = None
        self.idx = 0


class Lane:
    def __init__(self, prog, name, n):
        self.sems = [prog.new_sem(f"{name}{i}") for i in range(n)]
        self.cnt = 0
        self.last = [None] * n

    def next(self):
        i = self.cnt % len(self.sems)
        v = 16 * (self.cnt // len(self.sems) + 1)
        self.cnt += 1
        return i, self.sems[i], v, self.last[i]


class Prog:
    ENGS = ("pe", "act", "dve", "pool", "sp")

    def __init__(self, nc, stack):
        self.nc = nc
        self.stack = stack
        self.streams = {e: [] for e in self.ENGS}
        self.esem = {e: self.new_sem("e_" + e) for e in ("pe", "act", "dve", "pool")}
        self.lanes = {}

    def new_sem(self, name):
        return self.stack.enter_context(self.nc.semaphore(name))

    def lane(self, name, n=4):
        if name not in self.lanes:
            self.lanes[name] = Lane(self, name, n)
        return self.lanes[name]

    def sb(self, name, shape, dtype, stack=None):
        st = stack or self.stack
        self.nalloc = getattr(self, "nalloc", 0) + 1
        t = st.enter_context(self.nc.sbuf_tensor(f"sb{self.nalloc}_" + name, list(shape), dtype))
        return TT(t, name)

    def ps(self, name, shape, dtype=F32, stack=None):
        st = stack or self.stack
        self.nalloc = getattr(self, "nalloc", 0) + 1
        t = st.enter_context(self.nc.psum_tensor(f"ps{self.nalloc}_" + name, list(shape), dtype))
        r = TT(t, name)
        r.excl = True
        return r

    def dram(self, name, shape, dtype, kind="Internal"):
        t = self.nc.dram_tensor("dr_" + name, list(shape), dtype, kind=kind)
        return TT(t.ap(), name)

    @staticmethod
    def _norm(lst):
        out = []
        for x in lst:
            if x is None:
                continue
            if isinstance(x, tuple):
                out.append(x)
            else:
                out.append((x, None))
        return out

    def _deps(self, ins, reads, writes):
        deps = []

        def add(tok, raw):
            if tok is None or tok is ins:
                return
            if (not tok.is_dma) and (not ins.is_dma) and tok.eng == ins.eng:
                if ins.eng == "pe":
                    return
            deps.append(tok)

        reads = self._norm(reads)
        writes = self._norm(writes)
        for tt, lab in reads:
            if lab is None:
                add(tt.all.lastw, True)
                for r in tt.regs.values():
                    add(r.lastw, True)
                if tt.excl:
                    for t in tt.all.readers:
                        if t.is_dma or t.eng != ins.eng:
                            add(t, False)
            else:
                add(tt.reg(lab).lastw, True)
        for tt, lab in writes:
            regs = [tt.all] + list(tt.regs.values()) if lab is None else [tt.reg(lab)]
            for r in regs:
                add(r.lastw, False)
                for t in r.readers:
                    add(t, False)
        def addreader(r):
            if not ins.is_dma:
                r.readers[:] = [x for x in r.readers if x.is_dma or x.eng != ins.eng]
            r.readers.append(ins)

        for tt, lab in reads:
            if lab is None:
                addreader(tt.all)
                for r in tt.regs.values():
                    addreader(r)
            else:
                addreader(tt.reg(lab))
        for tt, lab in writes:
            regs = [tt.all] + list(tt.regs.values()) if lab is None else [tt.reg(lab)]
            for r in regs:
                r.lastw = ins
                r.readers = []
        seen = set()
        best = {}
        for d in deps:
            if d.is_dma:
                if id(d) not in seen:
                    seen.add(id(d))
                    ins.waits.append(d)
            else:
                b = best.get(d.eng)
                if b is None or d.idx > b.idx:
                    best[d.eng] = d
        for d in best.values():
            ins.waits.append(d)
            d.needed = True

    def op(self, eng, fn, reads=(), writes=()):
        ins = _Ins(eng, fn)
        ins.idx = len(self.streams[eng])
        self._deps(ins, reads, writes)
        self.streams[eng].append(ins)
        return ins

    def dma(self, eng, out, in_, reads=(), writes=(), lane="dma", **kw):
        ins = _Ins(eng, lambda e: e.dma_start(out=out, in_=in_, **kw))
        ins.is_dma = True
        ln = self.lane(lane)
        i, sem, val, prev = ln.next()
        ins.dsem, ins.dval = sem, val
        ln.last[i] = ins
        self._deps(ins, reads, writes)
        if prev is not None and all(prev is not w for w in ins.waits):
            ins.waits.append(prev)
        self.streams[eng].append(ins)
        return ins

    def barrier(self):
        toks = []
        for e in ("pe", "act", "dve", "pool"):
            for ins in reversed(self.streams[e]):
                if (not ins.is_dma) and ins.fn is not None:
                    toks.append(ins)
                    break
        for ln in self.lanes.values():
            for t in ln.last:
                if t is not None:
                    toks.append(t)
        for e in self.ENGS:
            self.wait_all(e, toks)

    def wait_all(self, eng, toks):
        ins = _Ins(eng, None)
        for t in toks:
            ins.waits.append(t)
            t.needed = True
        self.streams[eng].append(ins)

    def emit(self):
        nc = self.nc
        for e in ("pe", "act", "dve", "pool"):
            c = 0
            for ins in self.streams[e]:
                if ins.is_dma or ins.fn is None:
                    continue
                if ins.needed:
                    c += 1
                    ins.semval = c

        def tok_sem(t):
            if t.is_dma:
                return t.dsem, t.dval
            return self.esem[t.eng], t.semval

        def run(engname, eng):
            have = {}
            for ins in self.streams[engname]:
                for w in ins.waits:
                    sem, val = tok_sem(w)
                    k = id(sem)
                    if have.get(k, 0) >= val:
                        continue
                    have[k] = val
                    eng.wait_ge(sem, val)
                if ins.fn is None:
                    continue
                bi = ins.fn(eng)
                if ins.is_dma:
                    bi.then_inc(ins.dsem, 16)
                elif ins.needed:
                    bi.then_inc(self.esem[engname], 1)

        with nc.Block() as block:
            @block.tensor
            def _(e):
                run("pe", e)

            @block.scalar
            def _(e):
                run("act", e)

            @block.vector
            def _(e):
                run("dve", e)

            @block.gpsimd
            def _(e):
                run("pool", e)

            @block.sync
            def _(e):
                run("sp", e)


def mm(P, out, lhsT, rhs, start, stop, r, w):
    return P.op("pe", lambda e: e.matmul(out, lhsT, rhs, start=start, stop=stop,
                                         skip_group_check=True), r, w)


def tr(P, out, in_, ident, r, w):
    return P.op("pe", lambda e: e.transpose(out, in_, ident), r, w)


def act(P, out, in_, func, r, w, **kw):
    return P.op("act", lambda e: e.activation(out=out, in_=in_, func=func, **kw), r, w)


def ts(P, out, in0, s1, s2, op0, op1, r, w, eng="dve", **kw):
    if op1 is None:
        return P.op(eng, lambda e: e.tensor_scalar(out, in0, s1, None, op0, **kw), r, w)
    return P.op(eng, lambda e: e.tensor_scalar(out, in0, s1, s2, op0, op1, **kw), r, w)


def tt(P, out, in0, in1, op, r, w, eng="dve"):
    return P.op(eng, lambda e: e.tensor_tensor(out, in0, in1, op), r, w)


def stt(P, out, in0, scalar, in1, op0, op1, r, w):
    return P.op("dve", lambda e: e.scalar_tensor_tensor(out, in0, scalar, in1, op0, op1), r, w)


def cp(P, out, in_, r, w, eng="dve"):
    if eng == "act":
        return P.op("act", lambda e: e.copy(out, in_), r, w)
    return P.op(eng, lambda e: e.tensor_copy(out, in_), r, w)


def t5_bucket_np(dist):
    exact = 16
    d = dist.astype(np.float32)
    large = exact + (np.log(np.maximum(d, np.float32(exact)) / np.float32(exact)) / np.float32(math.log(128 / exact))
                     * np.float32(32 - exact)).astype(np.int32)
    large = np.minimum(large, 31)
    return np.where(dist < exact, dist, large)


class LayerIO:
    pass


def declare_io(nc, NSLOT, dbg):
    NB = 4 * NSLOT
    io = {}

    def inp(name, shape, dt=F32):
        io[name] = nc.dram_tensor(name, list(shape), dt, kind="ExternalInput").ap()

    inp("hfull", [NB, 128, D])
    inp("hown", [NSLOT, 128, D])
    inp("w_in", [D, 8008])
    inp("w_branch", [3, 512, D])
    inp("w_out", [D, D])
    inp("lbraw", [2, 512])
    inp("lsel", [128, 1])
    inp("norm_g", [1, 512])
    inp("sinks", [1, 8])
    inp("xa_wq", [D, D])
    inp("xa_wkv", [D, 2 * D])
    inp("xa_wo", [D, D])
    inp("mem", [256, D])
    inp("moe_wgwe", [D, 36])
    inp("moe_bgbe", [1, 36])
    inp("moe_w1", [32, D, 512])
    inp("moe_w3", [32, D, 512])
    inp("moe_w2", [32, 512, D])
    inp("ln_g", [3, D])
    inp("ln_b", [3, D])
    inp("ident", [128, 128])
    inp("tri64", [128, 64])
    inp("resetm", [128, 512])
    inp("cmask", [128, 512])
    inp("bA_diag", [128, 8, 128])
    inp("bA_prev", [128, 8, 128])
    inp("bC_diag", [128, 8, 128])
    inp("bC_prev", [128, 8, 128])
    inp("cm_diag", [128, 128])
    inp("cm_prevC", [128, 128])
    inp("relb31", [1, 16])
    inp("selv", [128, 40])
    io["hout"] = nc.dram_tensor("hout", [NSLOT, 128, D], F32, kind="ExternalOutput").ap()
    for name, shape in dbg.items():
        io[name] = nc.dram_tensor(name, list(shape), F32, kind="ExternalOutput").ap()
    return io


def build_layer(nc, NSLOT, dbg=None, stages=("S", "O", "T1", "T2", "T3")):
    dbg = dbg or {}
    NB = 4 * NSLOT
    S = 128 * NB
    io = declare_io(nc, NSLOT, dbg)
    root = ExitStack()
    with root:
        P = Prog(nc, root)
        out_toks = []

        def dbg_out(name, src_ap, src_tt, eng="sp"):
            if name in dbg:
                out_toks.append(P.dma(eng, io[name], src_ap, reads=[src_tt], lane="dbg"))

        kT_s = P.dram("kT_s", [NB, 128, 512], BF16)
        v1_s = P.dram("v1_s", [NB, 128, 520], BF16)
        cK_s = P.dram("cK_s", [NB, 128, 256], BF16)
        cV_s = P.dram("cV_s", [NB, 128, 130], BF16)
        h1_s = P.dram("h1_s", [NSLOT, 128, D], F32)

        ident = P.sb("ident", [128, 128], F32)
        identb = P.sb("identb", [128, 128], BF16)
        selv = P.sb("selv", [128, 40], F32)
        lb = P.sb("lb", [128, 4], F32)
        oml = P.sb("oml", [128, 4], F32)
        noml = P.sb("noml", [128, 4], F32)
        P.dma("sp", ident[:], io["ident"], writes=[ident], lane="c")
        P.dma("sp", selv[:], io["selv"], writes=[selv], lane="c")
        cp(P, identb[:], ident[:], [ident], [identb])
        with ExitStack() as st0:
            lbr = P.sb("lbr", [128, 2, 4], F32, st0)
            lselt = P.sb("lselt", [128, 1], F32, st0)
            P.dma("sp", lbr[:], io["lbraw"].rearrange("l (h p) -> p l h", p=128), writes=[lbr], lane="c",
                  allow_slow_non_contiguous=True)
            P.dma("sp", lselt[:], io["lsel"], writes=[lselt], lane="c")
            tt(P, lb[:], lbr[:, 1, :], lbr[:, 0, :], ALU.subtract, [lbr], [lb])
            act(P, lb[:], lb[:], AF.Sigmoid, [lb], [lb])
            ts(P, lb[:], lb[:], lselt[:, 0:1], None, ALU.mult, None, [lb, lselt], [lb])
            ts(P, lb[:], lb[:], 1e-30, None, ALU.max, None, [lb], [lb])
            ts(P, lb[:], lb[:], 1.0 - 1e-6, None, ALU.min, None, [lb], [lb])
            ts(P, oml[:], lb[:], -1.0, 1.0, ALU.mult, ALU.add, [lb], [oml])
            ts(P, noml[:], oml[:], -1.0, None, ALU.mult, None, [oml], [noml])
            P.barrier()

        sy = ExitStack()
        ybT = P.sb("ybT", [128, 4, 128 * NSLOT], BF16, sy)
        ikT = P.sb("ikT", [128, S], BF16, sy)

        if "S" in stages:
            pass_S(P, io, NSLOT, dict(kT_s=kT_s, v1_s=v1_s, cK_s=cK_s, cV_s=cV_s, ident=ident, identb=identb,
                                      selv=selv, lb=lb, oml=oml, noml=noml, ybT=ybT, ikT=ikT), dbg_out)
        P.barrier()
        ycT = P.sb("ycT", [128, 4, 128 * NSLOT], BF16, sy)
        yaT = P.sb("yaT", [128, 4, 128 * NSLOT], BF16, sy)
        if "O" in stages:
            pass_O(P, io, NSLOT, dict(kT_s=kT_s, v1_s=v1_s, cK_s=cK_s, cV_s=cV_s, ident=ident, identb=identb,
                                      selv=selv, ybT=ybT, ycT=ycT, yaT=yaT, ikT=ikT), dbg_out)
        for nm_, t_ in (("yaT", yaT), ("ybT", ybT), ("ycT", ycT)):
            dbg_out(nm_, t_[:], t_, eng="pool")
        P.barrier()
        if "T1" in stages:
            pass_T1(P, io, NSLOT, dict(ident=ident, identb=identb, ybT=ybT, ycT=ycT, yaT=yaT, h1_s=h1_s), dbg_out)
        P.barrier()
        sy.close()
        if "T2" in stages:
            pass_T23(P, io, NSLOT, dict(ident=ident, identb=identb, h1_s=h1_s), dbg_out, out_toks,
                     do_moe=("T3" in stages))
        P.wait_all("sp", out_toks)
        P.emit()
    return nc


WS_AK, WS_IK, WS_BQ, WS_BF, WS_CK, WS_AV, WS_BI, WS_CV, WS = 0, 512, 640, 1152, 1664, 1920, 2432, 2944, 3072


def load_w_cols(P, dst_tt, dst_off, w_ap, col0, n, lane="w"):
    src = w_ap.rearrange("(k p) c -> p k c", p=128)[:, :, col0:col0 + n]
    K = src.shape[1]
    return P.dma("pool", dst_tt[:, 0:K, dst_off:dst_off + n], src, writes=[dst_tt], lane=lane)


def pass_S(P, io, NSLOT, g, dbg_out):
    nc = P.nc
    NB = 4 * NSLOT
    ident, identb, selv = g["ident"], g["identb"], g["selv"]
    with ExitStack() as st:
        Wsh = P.sb("Wsh", [128, 8, WS], BF16, st)
        w_in = io["w_in"]
        load_w_cols(P, Wsh, WS_AK, w_in, OFF["a_k"], 512)
        load_w_cols(P, Wsh, WS_IK, w_in, OFF["i_k"], 64)
        load_w_cols(P, Wsh, WS_IK + 64, w_in, OFF["i_k"], 64)
        load_w_cols(P, Wsh, WS_BQ, w_in, OFF["b_q"], 512)
        load_w_cols(P, Wsh, WS_BF, w_in, OFF["b_f"], 512)
        load_w_cols(P, Wsh, WS_CK, w_in, OFF["c_k"], 64)
        load_w_cols(P, Wsh, WS_CK + 64, w_in, OFF["c_k"], 64)
        load_w_cols(P, Wsh, WS_CK + 128, w_in, OFF["c_k"] + 64, 64)
        load_w_cols(P, Wsh, WS_CK + 192, w_in, OFF["c_k"] + 64, 64)
        load_w_cols(P, Wsh, WS_AV, w_in, OFF["a_v"], 512)
        load_w_cols(P, Wsh, WS_BI, w_in, OFF["b_i"], 512)
        load_w_cols(P, Wsh, WS_CV, w_in, OFF["c_v"], 128)

        tri = P.sb("tri", [128, 64], F32, st)
        resetm = P.sb("resetm", [128, 512], F32, st)
        gain = P.sb("gain", [64, 512], F32, st)
        P.dma("sp", tri[:], io["tri64"], writes=[tri], lane="c")
        P.dma("sp", resetm[:], io["resetm"], writes=[resetm], lane="c")
        P.dma("sp", gain[:], io["norm_g"].partition_broadcast(64), writes=[gain], lane="c")

        hb = [P.sb(f"hb{i}", [128, D], F32, st) for i in range(2)]
        hT = [P.sb(f"hT{i}", [128, 8, 512], BF16, st) for i in range(2)]
        kst = P.sb("kst", [128, 4, 512], BF16, st)
        cst = P.sb("cst", [128, 4, 256], BF16, st)
        v1st = P.sb("v1st", [128, 4, 520], BF16, st)
        cvst = P.sb("cvst", [128, 4, 130], BF16, st)
        vch = P.sb("vch", [64, 8, 512], BF16, st)
        P.op("pool", lambda e: e.memset(v1st[:], 1.0), [], [v1st])
        P.op("pool", lambda e: e.memset(cvst[:], 1.0), [], [cvst])

        def T(name, dt=F32):
            return P.sb(name, [128, 512], dt, st)
        sig, ff, logf, kk, aa, am, E1, E2, qs, Ea, dl = [T(n) for n in
                                                         ("sig", "ff", "logf", "kk", "aa", "am", "E1", "E2", "qs", "Ea", "dl")]
        qaT = [T(f"qaT{h}", BF16) for h in range(4)]
        kbT = [T(f"kbT{h}", BF16) for h in range(4)]
        qeT = [T(f"qeT{h}", BF16) for h in range(4)]
        klT = [T(f"klT{h}", BF16) for h in range(4)]
        elast = P.sb("elast", [128, 4, 8], F32, st)
        state = P.sb("state", [128, 4, 128], F32, st)
        statb = P.sb("statb", [128, 4, 128], BF16, st)
        P.op("pool", lambda e: e.memset(state[:], 0.0), [], [state])
        P.op("pool", lambda e: e.memset(statb[:], 0.0), [], [statb])
        klsb = P.sb("klsb", [64, 512], BF16, st)
        atsb = P.sb("atsb", [64, 256], BF16, st)
        ybsel = P.sb("ybsel", [64, 2, 512], F32, st)
        ybsq = P.sb("ybsq", [64, 2, 512], F32, st)
        ybn = P.sb("ybn", [64, 2, 512], BF16, st)
        rms = P.sb("rms", [64, 8], F32, st)

        pj = [P.ps(f"pj{i}", [128, 512], F32, st) for i in range(2)]
        ptr = P.ps("ptr", [128, 1024], F32, st)
        pat = P.ps("pat", [128, 512], F32, st)
        pkl = P.ps("pkl", [128, 1024], BF16, st)
        po = P.ps("po", [128, 512], F32, st)
        pst = P.ps("pst", [128, 512], F32, st)
        pjc = [0]

        def next_pj():
            pjc[0] += 1
            return pj[pjc[0] % 2]

        for i in range(NSLOT):
            hTi = hT[i % 2]
            for r in range(4):
                blk = 4 * i + r
                hbi = hb[blk % 2]
                P.dma("sp", hbi[:], io["hfull"][blk], writes=[hbi], lane="h")
                for k in range(8):
                    tr(P, ptr[:, k * 128:(k + 1) * 128], hbi[:, k * 128:(k + 1) * 128], ident[:], [hbi, ident], [ptr])
                for half in range(2):
                    src = ptr[:, half * 512:(half + 1) * 512].rearrange("p (k t) -> p k t", k=4)
                    dst = hTi[:, half * 4:(half + 1) * 4, r * 128:(r + 1) * 128]
                    if half == 0:
                        cp(P, dst, src, [ptr], [(hTi, r)], eng="act")
                    else:
                        cp(P, dst, src, [ptr], [(hTi, r)], eng="dve")

            def proj_fm(col0, M=128):
                p = next_pj()
                for k in range(8):
                    mm(P, p[0:M, :], Wsh[:, k, col0:col0 + M], hTi[:, k, :], k == 0, k == 7, [Wsh, hTi], [p])
                return p

            for c in range(4):
                p = proj_fm(WS_AK + 128 * c)
                dst = kst[:, :, c * 128:(c + 1) * 128]
                src = p[:].rearrange("p (r t) -> p r t", r=4)
                cp(P, dst, src, [p], [kst], eng=("act" if c % 2 == 0 else "dve"))
            P.dma("sp", g["kT_s"][4 * i:4 * i + 4].rearrange("r p x -> p r x"), kst[:], reads=[kst],
                  writes=[(g["kT_s"], i)], lane="scw")
            p = proj_fm(WS_IK)
            cp(P, g["ikT"][:, 512 * i:512 * (i + 1)], p[:], [p], [(g["ikT"], i)], eng="act")
            for c in range(2):
                p = proj_fm(WS_CK + 128 * c)
                cp(P, cst[:, :, c * 128:(c + 1) * 128], p[:].rearrange("p (r t) -> p r t", r=4), [p], [cst], eng="dve")
            P.dma("sp", g["cK_s"][4 * i:4 * i + 4].rearrange("r p x -> p r x"), cst[:], reads=[cst],
                  writes=[(g["cK_s"], i)], lane="scw")

            for r in range(4):
                p = next_pj()
                for k in range(8):
                    mm(P, p[:, :], hTi[:, k, r * 128:(r + 1) * 128], Wsh[:, k, WS_AV:WS_AV + 512], k == 0, k == 7,
                       [Wsh, hTi], [p])
                cp(P, v1st[:, r, :].rearrange("p (h e) -> p h e", e=65)[:, :, 0:64],
                   p[:].rearrange("p (h e) -> p h e", e=64), [p], [v1st], eng="act")
                p = next_pj()
                for k in range(8):
                    mm(P, p[:, 0:128], hTi[:, k, r * 128:(r + 1) * 128], Wsh[:, k, WS_CV:WS_CV + 128], k == 0, k == 7,
                       [Wsh, hTi], [p])
                cp(P, cvst[:, r, :].rearrange("p (h e) -> p h e", e=65)[:, :, 0:64],
                   p[:, 0:128].rearrange("p (h e) -> p h e", e=64), [p], [cvst], eng="dve")
            P.dma("sp", g["v1_s"][4 * i:4 * i + 4].rearrange("r p x -> p r x"), v1st[:], reads=[v1st],
                  writes=[(g["v1_s"], i)], lane="scw")
            P.dma("sp", g["cV_s"][4 * i:4 * i + 4].rearrange("r p x -> p r x"), cvst[:], reads=[cvst],
                  writes=[(g["cV_s"], i)], lane="scw")
            for c in range(8):
                p = next_pj()
                for k in range(8):
                    mm(P, p[0:64, :], hTi[:, k, c * 64:(c + 1) * 64], Wsh[:, k, WS_BI:WS_BI + 512], k == 0, k == 7,
                       [Wsh, hTi], [p])
                cp(P, vch[:, c, :], p[0:64, :], [p], [vch], eng=("act" if c % 2 == 0 else "dve"))

            for hh in range(4):
                pf = proj_fm(WS_BF + 128 * hh)
                act(P, sig[:], pf[:], AF.Sigmoid, [pf], [sig])
                ts(P, ff[:], sig[:], g["oml"][:, hh:hh + 1], g["lb"][:, hh:hh + 1], ALU.mult, ALU.add,
                   [sig, g["oml"], g["lb"]], [ff])
                ts(P, kk[:], sig[:], g["noml"][:, hh:hh + 1], g["oml"][:, hh:hh + 1], ALU.mult, ALU.add,
                   [sig, g["oml"], g["noml"]], [kk])
                act(P, logf[:], ff[:], AF.Ln, [ff], [logf])
                P.op("dve", lambda e: e.tensor_tensor_scan(aa[:], resetm[:], logf[:], 0.0, ALU.mult, ALU.add),
                     [resetm, logf], [aa])
                aa3 = aa[:].rearrange("p (c t) -> p c t", t=64)
                alast_b = aa3[:, :, 63:64].to_broadcast([128, 8, 64])
                stt(P, am[:].rearrange("p (c t) -> p c t", t=64), alast_b, -0.5, aa3, ALU.mult, ALU.add, [aa], [am])
                act(P, E1[:], am[:], AF.Exp, [am], [E1])
                act(P, E2[:], am[:], AF.Exp, [am], [E2], scale=-1.0)
                pq = proj_fm(WS_BQ + 128 * hh)
                act(P, qs[:], pq[:], AF.Silu, [pq], [qs])
                tt(P, qaT[hh][:], qs[:], E1[:], ALU.mult, [qs, E1], [qaT[hh]])
                tt(P, kbT[hh][:], kk[:], E2[:], ALU.mult, [kk, E2], [kbT[hh]])
                act(P, Ea[:], aa[:], AF.Exp, [aa], [Ea])
                tt(P, qeT[hh][:], qs[:], Ea[:], ALU.mult, [qs, Ea], [qeT[hh]])
                tt(P, dl[:].rearrange("p (c t) -> p c t", t=64), alast_b, aa3, ALU.subtract, [aa], [dl])
                act(P, dl[:], dl[:], AF.Exp, [dl], [dl])
                tt(P, klT[hh][:], kk[:], dl[:], ALU.mult, [kk, dl], [klT[hh]])
                act(P, elast[:, hh, :], aa3[:, :, 63], AF.Exp, [aa], [elast])

            for c in range(8):
                cs = slice(c * 64, (c + 1) * 64)
                for hh in range(4):
                    tr(P, pkl[0:64, hh * 128:(hh + 1) * 128], klT[hh][:, cs], identb[:], [klT[hh], identb], [pkl])
                cp(P, klsb[:], pkl[0:64, 0:512], [pkl], [klsb], eng="act")
                for hh in range(4):
                    mm(P, pat[0:64, hh * 64:(hh + 1) * 64], kbT[hh][:, cs], qaT[hh][:, cs], True, True,
                       [kbT[hh], qaT[hh]], [pat])
                tt(P, atsb[:].rearrange("p (h t) -> p h t", h=4), pat[0:64, 0:256].rearrange("p (h t) -> p h t", h=4),
                   tri[0:64, :].unsqueeze(1).to_broadcast([64, 4, 64]), ALU.mult, [pat, tri], [atsb])
                for hh in range(4):
                    mm(P, po[0:64, hh * 128:(hh + 1) * 128], atsb[:, hh * 64:(hh + 1) * 64],
                       vch[:, c, hh * 128:(hh + 1) * 128], True, False, [atsb, vch], [po])
                    mm(P, po[0:64, hh * 128:(hh + 1) * 128], qeT[hh][:, cs], statb[:, hh, :], False, True,
                       [qeT[hh], statb], [po])
                for hh in range(4):
                    mm(P, pst[:, hh * 128:(hh + 1) * 128], klsb[:, hh * 128:(hh + 1) * 128],
                       vch[:, c, hh * 128:(hh + 1) * 128], True, True, [klsb, vch], [pst])
                for hh in range(4):
                    stt(P, state[:, hh, :], state[:, hh, :], elast[:, hh, c:c + 1], pst[:, hh * 128:(hh + 1) * 128],
                        ALU.mult, ALU.add, [state, elast, pst], [state])
                cp(P, statb[:], state[:], [state], [statb], eng="act")
                r, c2 = c // 2, c % 2
                if r == 0:
                    ts(P, ybsel[:, c2, :], po[0:64, :], selv[0:64, 0:1], None, ALU.mult, None, [po, selv], [ybsel])
                else:
                    stt(P, ybsel[:, c2, :], po[0:64, :], selv[0:64, r:r + 1], ybsel[:, c2, :], ALU.mult, ALU.add,
                        [po, selv, ybsel], [ybsel])

            act(P, ybsq[:], ybsel[:], AF.Square, [ybsel], [ybsq])
            P.op("dve", lambda e: e.tensor_reduce(rms[:], ybsq[:].rearrange("p c (h v) -> p (c h) v", v=128), AX.X, ALU.add),
                 [ybsq], [rms])
            ts(P, rms[:], rms[:], 1.0 / 128.0, RMS_EPS, ALU.mult, ALU.add, [rms], [rms])
            act(P, rms[:], rms[:], AF.Sqrt, [rms], [rms])
            P.op("dve", lambda e: e.reciprocal(rms[:], rms[:]), [rms], [rms])
            tt(P, ybsq[:].rearrange("p c (h v) -> p (c h) v", v=128), ybsel[:].rearrange("p c (h v) -> p (c h) v", v=128),
               rms[:].unsqueeze(2).to_broadcast([64, 8, 128]), ALU.mult, [ybsel, rms], [ybsq])
            tt(P, ybn[:], ybsq[:], gain[:].unsqueeze(1).to_broadcast([64, 2, 512]), ALU.mult, [ybsq, gain], [ybn])
            dbg_out(f"yb{i}", ybsq[:], ybsq)
            for c2 in range(2):
                for cc in range(4):
                    tr(P, pkl[:, (c2 * 4 + cc) * 64:(c2 * 4 + cc + 1) * 64], ybn[:, c2, cc * 128:(cc + 1) * 128],
                       identb[0:64, 0:64], [ybn, identb], [pkl])
            cp(P, g["ybT"][:, :, i * 128:(i + 1) * 128].rearrange("p c (a t) -> p a c t", a=2),
               pkl[:, 0:512].rearrange("p (a c t) -> p a c t", a=2, c=4), [pkl], [(g["ybT"], i)], eng="dve")


WO_AQ, WO_IQ, WO_BG, WO_CQ, WO_IW, WO = 0, 512, 1024, 1536, 2048, 2056
SV_SEL, SV_AD, SV_AP, SV_AM, SV_CD, SV_CP, SV_CM = 0, 4, 9, 14, 19, 24, 29
NIT = 18
TOPK = 256
SKIP = set()


def pass_O(P, io, NSLOT, g, dbg_out):
    NB = 4 * NSLOT
    S = 128 * NB
    ident, identb, selv, ikT = g["ident"], g["identb"], g["selv"], g["ikT"]
    with ExitStack() as st:
        Wown = P.sb("Wown", [128, 8, WO], BF16, st)
        w_in = io["w_in"]
        load_w_cols(P, Wown, WO_AQ, w_in, OFF["a_q"], 512)
        load_w_cols(P, Wown, WO_IQ, w_in, OFF["i_q"], 512)
        load_w_cols(P, Wown, WO_BG, w_in, OFF["b_g"], 512)
        load_w_cols(P, Wown, WO_CQ, w_in, OFF["c_q"], 512)
        load_w_cols(P, Wown, WO_IW, w_in, OFF["i_w"], 8)

        DA = P.sb("DA", [128, 8, 128], F32, st)
        PA = P.sb("PA", [128, 8, 128], F32, st)
        DC = P.sb("DC", [128, 8, 128], F32, st)
        PC = P.sb("PC", [128, 8, 128], F32, st)
        cmd = P.sb("cmd", [128, 128], F32, st)
        cmp_ = P.sb("cmp_", [128, 128], F32, st)
        cb = P.sb("cb", [128, 16], F32, st)
        esink = P.sb("esink", [128, 8], F32, st)
        cmask = P.sb("cmask", [128, 512], F32, st)
        I4 = P.sb("I4", [128, 4, 128], BF16, st)
        for t_, nm in ((DA, "bA_diag"), (PA, "bA_prev"), (DC, "bC_diag"), (PC, "bC_prev"), (cmd, "cm_diag"),
                       (cmp_, "cm_prevC"), (cmask, "cmask")):
            P.dma("sp", t_[:], io[nm], writes=[t_], lane="c")
        P.dma("sp", cb[:], io["relb31"].partition_broadcast(128), writes=[cb], lane="c")
        P.dma("sp", esink[:], io["sinks"].partition_broadcast(128), writes=[esink], lane="c")
        act(P, esink[:], esink[:], AF.Exp, [esink], [esink])
        cbb = cb[:, 0:8].unsqueeze(2).to_broadcast([128, 8, 128])
        tt(P, DA[:], DA[:], cbb, ALU.subtract, [DA, cb], [DA])
        tt(P, DA[:], DA[:], cmd[:].unsqueeze(1).to_broadcast([128, 8, 128]), ALU.add, [DA, cmd], [DA])
        tt(P, PA[:], PA[:], cbb, ALU.subtract, [PA, cb], [PA])
        tt(P, DC[:], DC[:], cmd[:].unsqueeze(1).to_broadcast([128, 8, 128]), ALU.add, [DC, cmd], [DC])
        tt(P, PC[:], PC[:], cmp_[:].unsqueeze(1).to_broadcast([128, 8, 128]), ALU.add, [PC, cmp_], [PC])
        for q in range(4):
            ts(P, I4[:, q, :], ident[:], -NEGB, None, ALU.mult, None, [ident], [I4])

        hbo = P.sb("hbo", [128, D], F32, st)
        hTo = P.sb("hTo", [128, 8, 128], BF16, st)
        qT = P.sb("qT", [128, 4, 128], BF16, st)
        iqT = P.sb("iqT", [128, 4, 128], BF16, st)
        cqT = P.sb("cqT", [128, 4, 128], BF16, st)
        sg = P.sb("sg", [128, 4, 128], BF16, st)
        iw = P.sb("iw", [128, 8], F32, st)
        absw = P.sb("absw", [128, 8], F32, st)
        sgn = P.sb("sgn", [128, 8], F32, st)
        Dg = P.sb("Dg", [128, 8, 128], BF16, st)
        Rb = [P.sb(f"Rb{i}", [128, 512], BF16, st) for i in range(2)]
        scores = P.sb("scores", [128, S], F32, st)
        maskneg = P.sb("maskneg", [128, S], BF16, st)
        bsv = P.sb("bsv", [128, 8], F32, st)
        wkt = P.sb("wkt", [128, NIT + 2], F32, st)
        kblk = [P.sb(f"kblk{i}", [128, 512], BF16, st) for i in range(3)]
        vblk = [P.sb(f"vblk{i}", [128, 520], BF16, st) for i in range(3)]
        ckb = [P.sb(f"ckb{i}", [128, 256], BF16, st) for i in range(2)]
        cvb = [P.sb(f"cvb{i}", [128, 130], BF16, st) for i in range(2)]
        tmpS = [P.sb(f"tmpS{i}", [128, 512], F32, st) for i in range(2)]
        Pt = [P.sb(f"Pt{i}", [128, 512], BF16, st) for i in range(2)]
        rec = P.sb("rec", [128, 8], F32, st)
        ytm = P.sb("ytm", [128, 8, 64], BF16, st)

        ptrA = P.ps("ptrAO", [128, 512], F32, st)
        pTb = P.ps("pTbO", [128, 1024], BF16, st)
        pq = [P.ps(f"pq{i}", [128, 512], F32, st) for i in range(2)]
        pS = [P.ps(f"pS{i}", [128, 512], F32, st) for i in range(2)]
        pO = [P.ps(f"pO{i}", [128, 512], F32, st) for i in range(2)]
        kcnt = [0]

        def finalize_y(dstT, i, extra=None):
            for bk in range(2):
                den = pO[bk][:, 0:260].rearrange("p (h e) -> p h e", e=65)[:, :, 64]
                if extra is not None:
                    tt(P, rec[:, bk::2], den, extra[:, bk::2], ALU.add, [pO[bk], extra], [rec])
                else:
                    cp(P, rec[:, bk::2], den, [pO[bk]], [rec])
            P.op("dve", lambda e: e.reciprocal(rec[:], rec[:]), [rec], [rec])
            for bk in range(2):
                tt(P, ytm[:, bk::2, :],
                   pO[bk][:, 0:260].rearrange("p (h e) -> p h e", e=65)[:, :, 0:64],
                   rec[:, bk::2].unsqueeze(2).to_broadcast([128, 4, 64]), ALU.mult, [pO[bk], rec], [ytm])
            ytf = ytm[:].rearrange("p h e -> p (h e)")
            for cc in range(4):
                tr(P, pTb[:, cc * 128:(cc + 1) * 128], ytf[:, cc * 128:(cc + 1) * 128], identb[:], [ytm, identb], [pTb])
            cp(P, dstT[:, :, i * 128:(i + 1) * 128], pTb[:, 0:512].rearrange("p (c t) -> p c t", c=4), [pTb],
               [(dstT, i)], eng="act")

        for i in range(NSLOT):
            P.dma("sp", hbo[:], io["hown"][i], writes=[hbo], lane="h")
            transpose_f32_block(P, hbo, ptrA, ident, [(lambda half: hTo[:, half * 4:(half + 1) * 4, :], hTo, "act")])

            def proj4(col0, bank):
                for c in range(4):
                    for k in range(8):
                        mm(P, bank[:, c * 128:(c + 1) * 128], Wown[:, k, col0 + c * 128:col0 + (c + 1) * 128],
                           hTo[:, k, :], k == 0, k == 7, [Wown, hTo], [bank])

            proj4(WO_AQ, pq[0])
            act(P, qT[:], pq[0][:].rearrange("p (c t) -> p c t", c=4), AF.Copy, [pq[0]], [qT], scale=0.125)
            proj4(WO_IQ, pq[1])
            act(P, iqT[:], pq[1][:].rearrange("p (c t) -> p c t", c=4), AF.Copy, [pq[1]], [iqT], scale=0.125)
            proj4(WO_CQ, pq[0])
            act(P, cqT[:], pq[0][:].rearrange("p (c t) -> p c t", c=4), AF.Copy, [pq[0]], [cqT], scale=0.125)
            proj4(WO_BG, pq[1])
            act(P, sg[:], pq[1][:].rearrange("p (c t) -> p c t", c=4), AF.Silu, [pq[1]], [sg])
            ybs = g["ybT"][:, :, i * 128:(i + 1) * 128]
            tt(P, ybs, ybs, sg[:], ALU.mult, [(g["ybT"], i), sg], [(g["ybT"], i)])
            for k in range(8):
                mm(P, pq[0][:, 0:8], hTo[:, k, :], Wown[:, k, WO_IW:WO_IW + 8], k == 0, k == 7, [Wown, hTo], [pq[0]])
            ts(P, iw[:], pq[0][:, 0:8], 8.0 ** -0.5, None, ALU.mult, None, [pq[0]], [iw])
            act(P, absw[:], iw[:], AF.Abs, [iw], [absw])
            ts(P, sgn[:], iw[:], 0.0, None, ALU.is_ge, None, [iw], [sgn])
            ts(P, sgn[:], sgn[:], 2.0, -1.0, ALU.mult, ALU.add, [sgn], [sgn])
            for h in range(8):
                ts(P, Dg[:, h, :], identb[:], sgn[:, h:h + 1], None, ALU.mult, None, [identb, sgn], [Dg])

            qs_ = [q for q in range(5) if 4 * i - 1 + q >= 0]
            if "swa" in SKIP:
                qs_ = []
            for qi, q in enumerate(qs_):
                blk = 4 * i - 1 + q
                ck, cv = ckb[qi % 2], cvb[qi % 2]
                if "swa_ck" not in SKIP:
                    P.dma("sp", ck[:], g["cK_s"][blk], reads=[(g["cK_s"], blk // 4)], writes=[ck], lane="scr")
                if "swa_cv" not in SKIP:
                    P.dma("sp", cv[:], g["cV_s"][blk], reads=[(g["cV_s"], blk // 4)], writes=[cv], lane="scr")
                for bk in range(2):
                    pb = 64 * bk
                    for hl in range(4 if "swa_mm" not in SKIP else 0):
                        kv = (2 * hl + bk) // 4
                        mm(P, pS[bk][:, hl * 128:(hl + 1) * 128], ck[pb:pb + 64, kv * 128:(kv + 1) * 128],
                           cqT[pb:pb + 64, hl, :], True, True, [ck, cqT], [pS[bk]])
                    tS, pt = tmpS[bk], Pt[bk]
                    if "swa_e" in SKIP:
                        continue
                    stt(P, tS[:], DC[:, bk * 4:(bk + 1) * 4, :].rearrange("p h t -> p (h t)"),
                        selv[:, SV_CD + q:SV_CD + q + 1], pS[bk][:], ALU.mult, ALU.add, [DC, selv, pS[bk]], [tS])
                    stt(P, tS[:], PC[:, bk * 4:(bk + 1) * 4, :].rearrange("p h t -> p (h t)"),
                        selv[:, SV_CP + q:SV_CP + q + 1], tS[:], ALU.mult, ALU.add, [PC, selv, tS], [tS])
                    act(P, pt[:], tS[:], AF.Exp, [tS, selv], [pt], bias=selv[:, SV_CM + q:SV_CM + q + 1])
                    for hl in range(4 if "swa_pv" not in SKIP else 0):
                        kv = (2 * hl + bk) // 4
                        mm(P, pO[bk][:, hl * 65:(hl + 1) * 65], pt[:, hl * 128:(hl + 1) * 128],
                           cv[:, kv * 65:(kv + 1) * 65], (qi == 0 and hl == 0), (qi == len(qs_) - 1 and hl == 3),
                           [pt, cv], [pO[bk]])
            if "swa" not in SKIP and "swa_fin" not in SKIP:
                finalize_y(g["ycT"], i, extra=esink)

            n = 512 * (i + 1)
            for c in range(i + 1 if "idx" not in SKIP else 0):
                acc = pS[c % 2]
                for h in range(8):
                    pb = 64 * (h % 2)
                    ph = pq[h % 2]
                    mm(P, ph[:, :], iqT[pb:pb + 64, h // 2, :], ikT[pb:pb + 64, c * 512:(c + 1) * 512], True, True,
                       [iqT, (ikT, c)], [ph])
                    act(P, Rb[h % 2][:], ph[:], AF.Relu, [ph, absw], [Rb[h % 2]], scale=absw[:, h:h + 1])
                    mm(P, acc[:, :], Dg[:, h, :], Rb[h % 2][:], h == 0, h == 7, [Dg, Rb[h % 2]], [acc])
                cp(P, scores[:, c * 512:(c + 1) * 512], acc[:], [acc], [scores], eng="act")
            B, w0, lo, mid, cnt, m_ = [bsv[:, k:k + 1] for k in range(6)]
            P.op("dve", lambda e, n=n, B=B: e.tensor_reduce(B, scores[:, 0:n], AX.X, ALU.max, apply_absolute_value=True),
                 [scores], [bsv])
            tt(P, scores[:, n - 512:n], scores[:, n - 512:n], cmask[:], ALU.add, [scores, cmask], [scores])
            ts(P, lo, B, -1.001, -1e-3, ALU.mult, ALU.add, [bsv], [bsv])
            ts(P, w0, B, 2.002, 2e-3, ALU.mult, ALU.add, [bsv], [bsv])
            for k in range(1, (NIT + 1) if "bis" not in SKIP else 1):
                f = 2.0 ** -k
                wk = wkt[:, k:k + 1]
                ts(P, wk, w0, f, None, ALU.mult, None, [bsv], [wkt])
                tt(P, mid, lo, wk, ALU.add, [bsv, wkt], [bsv])
                P.op("dve", lambda e, mid=mid, cnt=cnt, n=n: e.tensor_scalar(maskneg[:, 0:n], scores[:, 0:n], mid, None,
                                                                       ALU.is_ge, ALU.add, accum_out=cnt),
                     [scores, bsv], [maskneg, bsv])
                ts(P, m_, cnt, TOPK - 0.5, None, ALU.is_ge, None, [bsv], [bsv])
                stt(P, lo, m_, wk, lo, ALU.mult, ALU.add, [bsv, wkt], [bsv])
            ts(P, maskneg[:, 0:n], scores[:, 0:n], lo, None, ALU.is_ge, None, [scores, bsv], [maskneg])
            ts(P, maskneg[:, 0:n], maskneg[:, 0:n], -1.0, None, ALU.add, None, [maskneg], [maskneg])
            dbg_out(f"sc{i}", scores[:, 0:n], scores)
            dbg_out(f"lo{i}", bsv[:], bsv)

            nkb = 4 * i + 4
            for kb in range(nkb if "attn" not in SKIP else 0):
                kcnt[0] += 1
                kt, vt = kblk[kcnt[0] % 3], vblk[kcnt[0] % 3]
                P.dma("sp", kt[:], g["kT_s"][kb], reads=[(g["kT_s"], kb // 4)], writes=[kt], lane="scr")
                P.dma("sp", vt[:], g["v1_s"][kb], reads=[(g["v1_s"], kb // 4)], writes=[vt], lane="scr")
                q = kb - (4 * i - 1)
                for bk in range(2):
                    pb = 64 * bk
                    mm(P, pS[bk][:, :], maskneg[:, kb * 128:(kb + 1) * 128], I4[:].rearrange("p a t -> p (a t)"),
                       True, False, [maskneg, I4], [pS[bk]])
                    for hl in range(4):
                        mm(P, pS[bk][:, hl * 128:(hl + 1) * 128], kt[pb:pb + 64, hl * 128:(hl + 1) * 128],
                           qT[pb:pb + 64, hl, :], False, hl == 3, [kt, qT], [pS[bk]])
                    pt = Pt[bk]
                    if q >= 0:
                        tS = tmpS[bk]
                        stt(P, tS[:], DA[:, bk * 4:(bk + 1) * 4, :].rearrange("p h t -> p (h t)"),
                            selv[:, SV_AD + q:SV_AD + q + 1], pS[bk][:], ALU.mult, ALU.add, [DA, selv, pS[bk]], [tS])
                        stt(P, tS[:], PA[:, bk * 4:(bk + 1) * 4, :].rearrange("p h t -> p (h t)"),
                            selv[:, SV_AP + q:SV_AP + q + 1], tS[:], ALU.mult, ALU.add, [PA, selv, tS], [tS])
                        act(P, pt[:], tS[:], AF.Exp, [tS, selv], [pt], bias=selv[:, SV_AM + q:SV_AM + q + 1])
                    else:
                        act(P, pt[:], pS[bk][:], AF.Exp, [pS[bk]], [pt])
                    for hl in range(4):
                        h = 2 * hl + bk
                        mm(P, pO[bk][:, hl * 65:(hl + 1) * 65], pt[:, hl * 128:(hl + 1) * 128],
                           vt[:, h * 65:(h + 1) * 65], (kb == 0 and hl == 0), (kb == nkb - 1 and hl == 3),
                           [pt, vt], [pO[bk]])
            if "attn" not in SKIP:
                finalize_y(g["yaT"], i)


def load_bcast(P, dst_tt, src_row_ap, lane="c"):
    P.dma("sp", dst_tt[:], src_row_ap.partition_broadcast(128), writes=[dst_tt], lane=lane)


def layer_norm(P, x, out_ap, out_tt, gt, bt, stats, mv):
    for hlf in range(2):
        P.op("dve", lambda e, hlf=hlf: e.bn_stats(stats[:, hlf, :], x[:, hlf * 512:(hlf + 1) * 512]), [x], [stats])
    P.op("dve", lambda e: e.bn_aggr(mv[:, 0:2], stats[:].rearrange("p a b -> p (a b)")), [stats], [mv])
    ts(P, mv[:, 2:3], mv[:, 1:2], LN_EPS, None, ALU.add, None, [mv], [mv])
    act(P, mv[:, 2:3], mv[:, 2:3], AF.Sqrt, [mv], [mv])
    P.op("dve", lambda e: e.reciprocal(mv[:, 3:4], mv[:, 2:3]), [mv], [mv])
    ts(P, x[:], x[:], mv[:, 0:1], mv[:, 3:4], ALU.subtract, ALU.mult, [x, mv], [x])
    tt(P, x[:], x[:], gt[:], ALU.mult, [x, gt], [x])
    tt(P, out_ap, x[:], bt[:], ALU.add, [x, bt], [out_tt])


def transpose_f32_block(P, src_tt, ptrA, ident, dsts):
    for half in range(2):
        for k in range(4):
            kk = half * 4 + k
            tr(P, ptrA[:, k * 128:(k + 1) * 128], src_tt[:, kk * 128:(kk + 1) * 128], ident[:], [src_tt, ident], [ptrA])
        for (dst_fn, dtt, eng) in dsts:
            cp(P, dst_fn(half), ptrA[:].rearrange("p (k t) -> p k t", k=4), [ptrA], [dtt], eng=eng)


def transpose_bf_block(P, src_tt, src_ap, nchunk, pTb, identb, dst_ap, dst_tt, eng="act"):
    for c in range(nchunk):
        tr(P, pTb[:, c * 128:(c + 1) * 128], src_ap[:, c * 128:(c + 1) * 128], identb[:], [src_tt, identb], [pTb])
    cp(P, dst_ap, pTb[:, 0:nchunk * 128].rearrange("p (c t) -> p c t", c=nchunk), [pTb], [dst_tt], eng=eng)


def pass_T1(P, io, NSLOT, g, dbg_out):
    ident, identb = g["ident"], g["identb"]
    yT = [g["yaT"], g["ybT"], g["ycT"]]
    with ExitStack() as st:
        Wg = P.sb("Wg", [128, 8, 3072], BF16, st)
        Wbr = P.sb("Wbr", [128, 12, 1024], BF16, st)
        Wo = P.sb("Wo", [128, 8, 1024], BF16, st)
        for q in range(6):
            load_w_cols(P, Wg, q * 512, io["w_in"], OFF["gate"] + q * 512, 512)
        for gi in range(3):
            P.dma("pool", Wbr[:, gi * 4:(gi + 1) * 4, :], io["w_branch"][gi].rearrange("(c p) d -> p c d", p=128),
                  writes=[Wbr], lane="w")
        for q in range(2):
            load_w_cols(P, Wo, q * 512, io["w_out"], q * 512, 512)
        gt = P.sb("gt1", [128, D], F32, st)
        bt = P.sb("bt1", [128, D], F32, st)
        load_bcast(P, gt, io["ln_g"][0:1, :])
        load_bcast(P, bt, io["ln_b"][0:1, :])
        hbo = P.sb("hbo1", [128, D], F32, st)
        hTo = P.sb("hTo1", [128, 8, 128], BF16, st)
        sgate = P.sb("sgate", [128, 512], F32, st)
        merged = P.sb("merged", [128, D], F32, st)
        mtmp = P.sb("mtmp", [128, 512], F32, st)
        mbf = P.sb("mbf", [128, D], BF16, st)
        mT = P.sb("mT", [128, 8, 128], BF16, st)
        x1 = P.sb("x1", [128, D], F32, st)
        stats = P.sb("stats1", [128, 2, 6], F32, st)
        mv = P.sb("mv1", [128, 4], F32, st)
        ptrA = P.ps("ptrA1", [128, 512], F32, st)
        pTb = P.ps("pTb1", [128, 1024], BF16, st)
        pg = [P.ps(f"pg{i}", [128, 512], F32, st) for i in range(2)]
        pu = [P.ps(f"pu{i}", [128, 512], F32, st) for i in range(2)]
        cnt = [0]
        for i in range(NSLOT):
            P.dma("sp", hbo[:], io["hown"][i], writes=[hbo], lane="h")
            transpose_f32_block(P, hbo, ptrA, ident, [(lambda half: hTo[:, half * 4:(half + 1) * 4, :], hTo, "act")])
            for half in range(2):
                hs = slice(half * 512, (half + 1) * 512)
                for gi in range(3):
                    cnt[0] += 1
                    pgb, pub = pg[cnt[0] % 2], pu[cnt[0] % 2]
                    for k in range(8):
                        mm(P, pgb[:, :], hTo[:, k, :], Wg[:, k, gi * 1024 + half * 512:gi * 1024 + (half + 1) * 512],
                           k == 0, k == 7, [hTo, Wg], [pgb])
                    act(P, sgate[:], pgb[:], AF.Sigmoid, [pgb], [sgate])
                    for c in range(4):
                        mm(P, pub[:, :], yT[gi][:, c, i * 128:(i + 1) * 128], Wbr[:, gi * 4 + c, hs], c == 0, c == 3,
                           [(yT[gi], i), Wbr], [pub])
                    if gi == 0:
                        tt(P, merged[:, hs], pub[:], sgate[:], ALU.mult, [pub, sgate], [merged])
                    else:
                        tt(P, mtmp[:], pub[:], sgate[:], ALU.mult, [pub, sgate], [mtmp])
                        tt(P, merged[:, hs], merged[:, hs], mtmp[:], ALU.add, [merged, mtmp], [merged])
            cp(P, mbf[:], merged[:], [merged], [mbf], eng="act")
            transpose_bf_block(P, mbf, mbf[:], 8, pTb, identb, mT[:], mT, eng="dve")
            for half in range(2):
                hs = slice(half * 512, (half + 1) * 512)
                pb = pg[half]
                for k in range(8):
                    mm(P, pb[:, :], mT[:, k, :], Wo[:, k, hs], k == 0, k == 7, [mT, Wo], [pb])
                stt(P, x1[:, hs], hbo[:, hs], ALPHA, pb[:], ALU.mult, ALU.add, [hbo, pb], [x1])
            layer_norm(P, x1, x1[:], x1, gt, bt, stats, mv)
            dbg_out(f"h1_{i}", x1[:], x1)
            P.dma("sp", g["h1_s"][i], x1[:], reads=[x1], writes=[(g["h1_s"], i)], lane="scw")


def pass_T23(P, io, NSLOT, g, dbg_out, out_toks, do_moe=True):
    ident, identb = g["ident"], g["identb"]
    NT = 128 * NSLOT
    with ExitStack() as st:
        H2 = P.sb("H2", [128, NSLOT, D], F32, st)
        h2T = P.sb("h2T", [128, 8, NT], BF16, st)
        gate = P.sb("gate", [128, NSLOT, 32], F32, st)
        stats = P.sb("stats2", [128, 2, 6], F32, st)
        mv = P.sb("mv2", [128, 4], F32, st)
        with ExitStack() as s2:
            Wq = P.sb("Wq", [128, 8, 1024], BF16, s2)
            Wxo = P.sb("Wxo", [128, 8, 1024], BF16, s2)
            Wr = P.sb("Wr", [128, 8, 36], F32, s2)
            br = P.sb("br", [128, 36], F32, s2)
            KT = P.sb("KT", [128, 8, 256], BF16, s2)
            V1m = P.sb("V1m", [128, 2, 4, 257], BF16, s2)
            gt = P.sb("gt2", [128, D], F32, s2)
            bt = P.sb("bt2", [128, D], F32, s2)
            for q in range(2):
                load_w_cols(P, Wq, q * 512, io["xa_wq"], q * 512, 512)
                load_w_cols(P, Wxo, q * 512, io["xa_wo"], q * 512, 512)
            P.dma("sp", Wr[:], io["moe_wgwe"].rearrange("(k p) c -> p k c", p=128), writes=[Wr], lane="c")
            load_bcast(P, br, io["moe_bgbe"])
            load_bcast(P, gt, io["ln_g"][1:2, :])
            load_bcast(P, bt, io["ln_b"][1:2, :])
            ptrA = P.ps("ptrA2", [128, 512], F32, s2)
            pTb = P.ps("pTb2", [128, 1024], BF16, s2)
            pS = [P.ps(f"pS2{i}", [128, 512], F32, s2) for i in range(2)]
            pX = [P.ps(f"pX{i}", [128, 512], F32, s2) for i in range(4)]
            hb1 = P.sb("hb1", [128, D], F32, s2)
            h1T = P.sb("h1T", [128, 8, 128], BF16, s2)
            with ExitStack() as s3:
                Wkv = P.sb("Wkv", [128, 8, 2048], BF16, s3)
                memT = P.sb("memT", [128, 8, 256], BF16, s3)
                for q in range(4):
                    load_w_cols(P, Wkv, q * 512, io["xa_wkv"], q * 512, 512)
                P.op("pool", lambda e: e.memset(V1m[:], 1.0), [], [V1m])
                for mb in range(2):
                    P.dma("sp", hb1[:], io["mem"][mb * 128:(mb + 1) * 128, :], writes=[hb1], lane="h")
                    transpose_f32_block(P, hb1, ptrA, ident,
                                        [(lambda half, mb=mb: memT[:, half * 4:(half + 1) * 4, mb * 128:(mb + 1) * 128],
                                          memT, "act")])
                for c in range(8):
                    pb = pX[c % 4]
                    for k in range(8):
                        mm(P, pb[:, 0:256], Wkv[:, k, c * 128:(c + 1) * 128], memT[:, k, :], k == 0, k == 7,
                           [Wkv, memT], [pb])
                    cp(P, KT[:, c, :], pb[:, 0:256], [pb], [KT], eng=("act" if c % 2 else "dve"))
                for mb in range(2):
                    for half in range(2):
                        pb = pX[(mb * 2 + half) % 4]
                        for k in range(8):
                            mm(P, pb[:, :], memT[:, k, mb * 128:(mb + 1) * 128],
                               Wkv[:, k, 1024 + half * 512:1024 + (half + 1) * 512], k == 0, k == 7, [Wkv, memT], [pb])
                        cp(P, V1m[:, mb, half * 2:(half + 1) * 2, 0:256], pb[:].rearrange("p (h e) -> p h e", h=2),
                           [pb], [V1m], eng=("act" if half else "dve"))
            P.barrier()
            xqT = P.sb("xqT", [128, 8, 128], BF16, s2)
            Ptm = [P.sb(f"Ptm{i}", [128, 512], BF16, s2) for i in range(2)]
            rec = P.sb("rec2", [128, 4], F32, s2)
            xo = P.sb("xo", [128, D], BF16, s2)
            xoT = P.sb("xoT", [128, 8, 128], BF16, s2)
            x2 = P.sb("x2", [128, D], F32, s2)
            h2Tf = P.sb("h2Tf", [128, 8, 128], F32, s2)
            rl = P.sb("rl", [128, 36], F32, s2)
            rt = P.sb("rt", [128, 64], F32, s2)
            elm = P.sb("elm", [128, 4, 8], F32, s2)
            oh1 = P.sb("oh1", [128, 32], F32, s2)
            oh2 = P.sb("oh2", [128, 32], F32, s2)
            for i in range(NSLOT):
                P.dma("sp", hb1[:], g["h1_s"][i], reads=[(g["h1_s"], i)], writes=[hb1], lane="h")
                transpose_f32_block(P, hb1, ptrA, ident, [(lambda half: h1T[:, half * 4:(half + 1) * 4, :], h1T, "act")])
                for c in range(8):
                    pb = pX[c // 4]
                    for k in range(8):
                        mm(P, pb[:, (c % 4) * 128:(c % 4 + 1) * 128], Wq[:, k, c * 128:(c + 1) * 128], h1T[:, k, :],
                           k == 0, k == 7, [Wq, h1T], [pb])
                for b2 in range(2):
                    act(P, xqT[:, b2 * 4:(b2 + 1) * 4, :], pX[b2][:].rearrange("p (c t) -> p c t", c=4), AF.Copy,
                        [pX[b2]], [xqT], scale=1.0 / 16.0)
                for mb in range(2):
                    for h in range(4):
                        for dc in range(2):
                            mm(P, pS[mb][:, h * 128:(h + 1) * 128], KT[:, 2 * h + dc, mb * 128:(mb + 1) * 128],
                               xqT[:, 2 * h + dc, :], dc == 0, dc == 1, [KT, xqT], [pS[mb]])
                    act(P, Ptm[mb][:], pS[mb][:], AF.Exp, [pS[mb]], [Ptm[mb]])
                for h in range(4):
                    for mb in range(2):
                        mm(P, pX[h][:, 0:257], Ptm[mb][:, h * 128:(h + 1) * 128], V1m[:, mb, h, :], mb == 0, mb == 1,
                           [Ptm[mb], V1m], [pX[h]])
                for h in range(4):
                    cp(P, rec[:, h:h + 1], pX[h][:, 256:257], [pX[h]], [rec])
                P.op("dve", lambda e: e.reciprocal(rec[:], rec[:]), [rec], [rec])
                for h in range(4):
                    ts(P, xo[:, h * 256:(h + 1) * 256], pX[h][:, 0:256], rec[:, h:h + 1], None, ALU.mult, None,
                       [pX[h], rec], [xo], eng="dve")
                transpose_bf_block(P, xo, xo[:], 8, pTb, identb, xoT[:], xoT, eng="act")
                for half in range(2):
                    hs = slice(half * 512, (half + 1) * 512)
                    pb = pS[half]
                    for k in range(8):
                        mm(P, pb[:, :], xoT[:, k, :], Wxo[:, k, hs], k == 0, k == 7, [xoT, Wxo], [pb])
                    stt(P, x2[:, hs], hb1[:, hs], ALPHA, pb[:], ALU.mult, ALU.add, [hb1, pb], [x2])
                layer_norm(P, x2, H2[:, i, :], H2, gt, bt, stats, mv)
                dbg_out(f"h2_{i}", H2[:, i, :], H2)
                for half in range(2):
                    for k in range(4):
                        kk = half * 4 + k
                        tr(P, ptrA[:, k * 128:(k + 1) * 128], H2[:, i, kk * 128:(kk + 1) * 128], ident[:], [H2, ident],
                           [ptrA])
                    cp(P, h2T[:, half * 4:(half + 1) * 4, i * 128:(i + 1) * 128],
                       ptrA[:].rearrange("p (k t) -> p k t", k=4), [ptrA], [h2T], eng="act")
                    cp(P, h2Tf[:, half * 4:(half + 1) * 4, :], ptrA[:].rearrange("p (k t) -> p k t", k=4), [ptrA],
                       [h2Tf], eng="act")
                pr = pX[0]
                for k in range(8):
                    mm(P, pr[:, 0:36], h2Tf[:, k, :], Wr[:, k, :], k == 0, k == 7, [h2Tf, Wr], [pr])
                tt(P, rl[:], pr[:, 0:36], br[:], ALU.add, [pr, br], [rl])
                gmax, gsum, pgrp, m1, m2, w1_, w2_ = [rt[:, k:k + 1] for k in range(7)]
                goh = rt[:, 8:12]
                gex = rt[:, 12:16]
                P.op("dve", lambda e: e.tensor_reduce(gmax, rl[:, 0:4], AX.X, ALU.max), [rl], [rt])
                ts(P, goh, rl[:, 0:4], gmax, None, ALU.is_ge, None, [rl, rt], [rt])
                ts(P, gex, rl[:, 0:4], gmax, None, ALU.subtract, None, [rl, rt], [rt])
                act(P, gex, gex, AF.Exp, [rt], [rt])
                P.op("dve", lambda e: e.tensor_reduce(gsum, gex, AX.X, ALU.add), [rt], [rt])
                P.op("dve", lambda e: e.reciprocal(pgrp, gsum), [rt], [rt])
                ts(P, rt[:, 16:20], goh, 1e9, -1e9, ALU.mult, ALU.add, [rt], [rt])
                tt(P, elm[:], rl[:, 4:36].rearrange("p (g e) -> p g e", g=4),
                   rt[:, 16:20].unsqueeze(2).to_broadcast([128, 4, 8]), ALU.add, [rl, rt], [elm])
                elf = elm[:].rearrange("p g e -> p (g e)")
                P.op("dve", lambda e: e.tensor_reduce(m1, elf, AX.X, ALU.max), [elm], [rt])
                ts(P, oh1[:], elf, m1, None, ALU.is_ge, None, [elm, rt], [oh1])
                stt(P, elf, oh1[:], -2e9, elf, ALU.mult, ALU.add, [oh1, elm], [elm])
                P.op("dve", lambda e: e.tensor_reduce(m2, elf, AX.X, ALU.max), [elm], [rt])
                ts(P, oh2[:], elf, m2, None, ALU.is_ge, None, [elm, rt], [oh2])
                tt(P, w2_, m2, m1, ALU.subtract, [rt], [rt])
                act(P, w2_, w2_, AF.Exp, [rt], [rt])
                ts(P, w1_, w2_, 1.0, None, ALU.add, None, [rt], [rt])
                P.op("dve", lambda e: e.reciprocal(w1_, w1_), [rt], [rt])
                tt(P, w2_, w2_, w1_, ALU.mult, [rt], [rt])
                tt(P, w1_, w1_, pgrp, ALU.mult, [rt], [rt])
                tt(P, w2_, w2_, pgrp, ALU.mult, [rt], [rt])
                ts(P, gate[:, i, :], oh1[:], w1_, None, ALU.mult, None, [oh1, rt], [gate])
                stt(P, gate[:, i, :], oh2[:], w2_, gate[:, i, :], ALU.mult, ALU.add, [oh2, rt, gate], [gate])
                dbg_out(f"gate{i}", gate[:, i, :], gate)
                ts(P, H2[:, i, :], H2[:, i, :], ALPHA, None, ALU.mult, None, [H2], [H2])

        P.barrier()
        with ExitStack() as s4:
            gt = P.sb("gt3", [128, D], F32, s4)
            bt = P.sb("bt3", [128, D], F32, s4)
            load_bcast(P, gt, io["ln_g"][2:3, :])
            load_bcast(P, bt, io["ln_b"][2:3, :])
            if do_moe:
                TG = 128 * min(4, NSLOT)
                NG = NT // TG
                W1 = [P.sb(f"W1_{i}", [128, 8, 512], BF16, s4) for i in range(2)]
                W3 = [P.sb(f"W3_{i}", [128, 8, 512], BF16, s4) for i in range(2)]
                W2 = [P.sb(f"W2_{i}", [128, 4, 1024], BF16, s4) for i in range(2)]
                sil = [P.sb(f"sil{i}", [128, 512], F32, s4) for i in range(2)]
                hdT = [P.sb(f"hdT{i}", [128, 4, 512], BF16, s4) for i in range(2)]
                p1 = [P.ps(f"p1_{i}", [128, 512], F32, s4) for i in range(2)]
                p3 = [P.ps(f"p3_{i}", [128, 512], F32, s4) for i in range(2)]
                py = [P.ps(f"py{i}", [128, 512], F32, s4) for i in range(2)]
                cc = [0]
                for e_ in range(32):
                    w1t, w3t, w2t = W1[e_ % 2], W3[e_ % 2], W2[e_ % 2]
                    P.dma("pool", w1t[:], io["moe_w1"][e_].rearrange("(k p) f -> p k f", p=128), writes=[w1t], lane="w")
                    P.dma("pool", w3t[:], io["moe_w3"][e_].rearrange("(k p) f -> p k f", p=128), writes=[w3t], lane="w")
                    P.dma("pool", w2t[:], io["moe_w2"][e_].rearrange("(c p) d -> p c d", p=128), writes=[w2t], lane="w")
                    for gi in range(NG):
                        hd = hdT[gi % 2]
                        tok = slice(gi * TG, (gi + 1) * TG)
                        for fc in range(4):
                            cc[0] += 1
                            a1, a3, sl = p1[cc[0] % 2], p3[cc[0] % 2], sil[cc[0] % 2]
                            for k in range(8):
                                mm(P, a1[:, 0:TG], w1t[:, k, fc * 128:(fc + 1) * 128], h2T[:, k, tok], k == 0, k == 7,
                                   [w1t, h2T], [a1])
                            for k in range(8):
                                mm(P, a3[:, 0:TG], w3t[:, k, fc * 128:(fc + 1) * 128], h2T[:, k, tok], k == 0, k == 7,
                                   [w3t, h2T], [a3])
                            act(P, sl[:, 0:TG], a1[:, 0:TG], AF.Silu, [a1], [sl])
                            tt(P, hd[:, fc, 0:TG], a3[:, 0:TG], sl[:, 0:TG], ALU.mult, [a3, sl], [hd])
                        for b_ in range(TG // 128):
                            slot = gi * (TG // 128) + b_
                            for half in range(2):
                                hs = slice(half * 512, (half + 1) * 512)
                                yb = py[half]
                                for fc in range(4):
                                    mm(P, yb[:, :], hd[:, fc, b_ * 128:(b_ + 1) * 128], w2t[:, fc, hs], fc == 0, fc == 3,
                                       [hd, w2t], [yb])
                                stt(P, H2[:, slot, hs], yb[:], gate[:, slot, e_:e_ + 1], H2[:, slot, hs], ALU.mult,
                                    ALU.add, [yb, gate, H2], [H2])
            for i in range(NSLOT):
                for hlf in range(2):
                    P.op("dve", lambda e, hlf=hlf, i=i: e.bn_stats(stats[:, hlf, :], H2[:, i, hlf * 512:(hlf + 1) * 512]),
                         [H2], [stats])
                P.op("dve", lambda e: e.bn_aggr(mv[:, 0:2], stats[:].rearrange("p a b -> p (a b)")), [stats], [mv])
                ts(P, mv[:, 2:3], mv[:, 1:2], LN_EPS, None, ALU.add, None, [mv], [mv])
                act(P, mv[:, 2:3], mv[:, 2:3], AF.Sqrt, [mv], [mv])
                P.op("dve", lambda e: e.reciprocal(mv[:, 3:4], mv[:, 2:3]), [mv], [mv])
                ts(P, H2[:, i, :], H2[:, i, :], mv[:, 0:1], mv[:, 3:4], ALU.subtract, ALU.mult, [H2, mv], [H2])
                tt(P, H2[:, i, :], H2[:, i, :], gt[:], ALU.mult, [H2, gt], [H2])
                tt(P, H2[:, i, :], H2[:, i, :], bt[:], ALU.add, [H2, bt], [H2])
                out_toks.append(P.dma("sp", io["hout"][i], H2[:, i, :], reads=[H2], lane="out"))


def core_consts(j, rel_bias):
    f = np.float32
    c = {}
    c["ident"] = np.eye(128, dtype=f)
    s64 = np.arange(64)
    tri = (s64[:, None] <= s64[None, :]).astype(f)
    c["tri64"] = np.concatenate([tri, tri], axis=0)
    rm = np.ones((128, 512), f)
    rm[:, ::64] = 0.0
    c["resetm"] = rm
    t = np.arange(128)[:, None]
    s = np.arange(128)[None, :]
    cm = np.zeros((128, 4, 128), f)
    for r in range(4):
        cm[:, r, :] = np.where((j - r) * 128 + t - s >= 0, 0.0, -1e30)
    c["cmask"] = cm.reshape(128, 512)
    ss = np.arange(128)[:, None]
    tq = np.arange(128)[None, :]
    d_diag = np.maximum(tq - ss, 0)
    d_prev = tq + 128 - ss
    bd = t5_bucket_np(d_diag.astype(np.int32))
    bp = t5_bucket_np(d_prev.astype(np.int32))
    perm = np.array([0, 2, 4, 6, 1, 3, 5, 7])
    c["bA_diag"] = np.ascontiguousarray(rel_bias[bd][:, :, perm].transpose(0, 2, 1)).astype(f)
    c["bA_prev"] = np.ascontiguousarray(rel_bias[bp][:, :, perm].transpose(0, 2, 1)).astype(f)
    c["bC_diag"] = np.ascontiguousarray(rel_bias[bd][:, :, 8 + perm].transpose(0, 2, 1)).astype(f)
    c["bC_prev"] = np.ascontiguousarray(rel_bias[bp][:, :, 8 + perm].transpose(0, 2, 1)).astype(f)
    c["cm_diag"] = np.where(tq >= ss, 0.0, NEGB).astype(f)
    c["cm_prevC"] = np.where(ss > tq, 0.0, NEGB).astype(f)
    c["relb31"] = np.ascontiguousarray(rel_bias[31:32, np.concatenate([perm, 8 + perm])]).astype(f)
    sv = np.zeros((128, 40), f)
    sv[:, SV_SEL + j] = 1.0
    for q in range(5):
        r = q - 1
        isD = float(r == j)
        isP = float(r == j - 1)
        sv[:, SV_AD + q] = isD
        sv[:, SV_AP + q] = isP
        sv[:, SV_AM + q] = NEGB if r > j else 0.0
        sv[:, SV_CD + q] = isD
        sv[:, SV_CP + q] = isP
        sv[:, SV_CM + q] = 0.0 if (isD or isP) else NEGB
    c["selv"] = sv
    return c


def layer_inputs(l, inputs, hfull_b, NSLOT):
    f = np.float32
    maps = []
    shared = dict(
        w_in=inputs["w_in"][l], w_branch=inputs["w_branch"][l], w_out=inputs["w_out"][l],
        lbraw=inputs["hgrn_lb"], lsel=np.full((128, 1), float(l > 0), f),
        norm_g=inputs["hgrn_norm_g"][l].reshape(1, 512), sinks=inputs["c_sinks"][l].reshape(1, 8),
        xa_wq=inputs["xa_wq"][l], xa_wkv=inputs["xa_wkv"][l], xa_wo=inputs["xa_wo"][l],
        moe_wgwe=np.concatenate([inputs["moe_wg"][l], inputs["moe_we"][l]], axis=1),
        moe_bgbe=np.concatenate([inputs["moe_bg"][l], inputs["moe_be"][l]]).reshape(1, 36),
        moe_w1=inputs["moe_w1"][l], moe_w3=inputs["moe_w3"][l], moe_w2=inputs["moe_w2"][l],
        ln_g=inputs["ln_g"][l], ln_b=inputs["ln_b"][l],
    )
    shared = {k: np.ascontiguousarray(v, dtype=f) for k, v in shared.items()}
    NB = 4 * NSLOT
    for core in range(8):
        b, j = core // 4, core % 4
        hf = np.ascontiguousarray(hfull_b[b], dtype=f).reshape(NB, 128, D)
        m = dict(shared)
        m["hfull"] = hf
        m["hown"] = np.ascontiguousarray(hf[j::4])
        m["mem"] = np.ascontiguousarray(inputs["mem"][b], dtype=f)
        m.update(core_consts(j, np.asarray(inputs["rel_bias"], dtype=f)))
        maps.append(m)
    return maps


_NC_CACHE = {}


def get_program(NSLOT, dbg=None, stages=("S", "O", "T1", "T2", "T3")):
    key = (NSLOT, tuple(sorted((dbg or {}).items())), tuple(stages))
    if key not in _NC_CACHE:
        nc = bass.Bass("TRN2", target_bir_lowering=False)
        build_layer(nc, NSLOT, dbg, stages)
        _NC_CACHE[key] = nc
    return _NC_CACHE[key]


def run_layers(inputs, NSLOT, depth=2):
    f = np.float32
    x = np.asarray(inputs["x"], dtype=f)
    S = 512 * NSLOT
    h = [x[0, :S], x[1, :S]]
    nc = get_program(NSLOT)
    for l in range(depth):
        maps = layer_inputs(l, inputs, h, NSLOT)
        res = run_bass_kernel_spmd(nc, maps, core_ids=list(range(8)))
        newh = []
        for b in range(2):
            hb = np.zeros((4 * NSLOT, 128, D), f)
            for j in range(4):
                hb[j::4] = res.results[b * 4 + j]["hout"]
            newh.append(hb.reshape(S, D))
        h = newh
    return np.stack(h, axis=0)


def kernel(**inputs):
    inputs = {k: np.asarray(v) for k, v in inputs.items()}
    return run_layers(inputs, 16).astype(np.float32)
```
